# Optimizing a Trainium2 kernel written in Bass

```python
import jax, jax.numpy as jnp
from jax import lax
import numpy as np

D_MODEL = 1024
BATCH = 4
SEQ = 8192
DEPTH = 2

EPS = 1e-6
POOL_WINDOWS = (2, 4, 8, 16)
POOL_GROUPS = len(POOL_WINDOWS)
POOL_GROUP_DIM = D_MODEL // 8
POOL_DIM = POOL_GROUPS * POOL_GROUP_DIM
ATT_HEADS = 8
ATT_KV_HEADS = 2
HEAD_DIM = D_MODEL // 16
ATT_DIM = ATT_HEADS * HEAD_DIM
KV_DIM = ATT_KV_HEADS * HEAD_DIM
IDX_HEADS = 4
IDX_DIM = 64
TOPK_MAX = 256
Q_BLOCK = 128
ROPE_THETA = 500000.0
ROPE_DIM = HEAD_DIM // 4
HGRN_HEADS = 8
HGRN_DK = D_MODEL // HGRN_HEADS
HGRN_DV = D_MODEL // HGRN_HEADS
HGRN_WIDTH = HGRN_HEADS * HGRN_DK
HGRN_CHUNK = 64
D_FF = ((8 * D_MODEL // 3 + 255) // 256) * 256

AB_IN = POOL_DIM + ATT_DIM + 2 * KV_DIM + IDX_HEADS * IDX_DIM + IDX_DIM + IDX_HEADS
C_IN = 4 * HGRN_WIDTH
N_EVEN = (DEPTH + 1) // 2
N_ODD = DEPTH // 2

kernel_name = "hybrid_pool_dsa_hgrn2_trunk"


def rmsnorm(x, g):
    xf = x.astype(jnp.float32)
    y = xf * lax.rsqrt(jnp.mean(xf * xf, axis=-1, keepdims=True) + EPS)
    return (y * g.astype(jnp.float32)).astype(x.dtype)


def rope_tables(positions):
    inv = ROPE_THETA ** (-jnp.arange(0, ROPE_DIM, 2, dtype=jnp.float32) / ROPE_DIM)
    ang = positions.astype(jnp.float32)[..., None] * inv
    return jnp.cos(ang)[:, :, None, :], jnp.sin(ang)[:, :, None, :]


def partial_rope(x, cos, sin):
    half = ROPE_DIM // 2
    xr = x[..., :ROPE_DIM].astype(jnp.float32)
    x1, x2 = xr[..., :half], xr[..., half:]
    rot = jnp.concatenate([x1 * cos - x2 * sin, x2 * cos + x1 * sin], axis=-1)
    return jnp.concatenate([rot.astype(x.dtype), x[..., ROPE_DIM:]], axis=-1)


def pool_mixer(u, w_pool, pool_scale):
    L = u.shape[1]
    t = jnp.arange(L)
    outs = []
    for g, w in enumerate(POOL_WINDOWS):
        ug = u[..., g * POOL_GROUP_DIM:(g + 1) * POOL_GROUP_DIM].astype(jnp.float32)
        cs = jnp.cumsum(ug, axis=1)
        lag = jnp.pad(cs[:, :-w], ((0, 0), (w, 0), (0, 0)))
        cnt = jnp.minimum(t + 1, w).astype(jnp.float32)[None, :, None]
        pooled = (cs - lag) / cnt - ug
        outs.append(jnp.einsum('blc,cd->bld', pooled.astype(u.dtype), w_pool[g]))
    return jnp.concatenate(outs, axis=-1) * pool_scale


def sparse_attention(q, k, v, qi, ki, wi):
    B, L = q.shape[0], q.shape[1]
    topk = min(TOPK_MAX, L // 4)
    n_blocks = L // Q_BLOCK
    rep = ATT_HEADS // ATT_KV_HEADS
    key_pos = jnp.arange(L)
    scale = HEAD_DIM ** -0.5
    ki32 = ki.astype(jnp.float32)

    def block(i):
        start = i * Q_BLOCK
        qb = lax.dynamic_slice_in_dim(q, start, Q_BLOCK, axis=1)
        qib = lax.dynamic_slice_in_dim(qi, start, Q_BLOCK, axis=1).astype(jnp.float32)
        wib = lax.dynamic_slice_in_dim(wi, start, Q_BLOCK, axis=1).astype(jnp.float32)
        qpos = start + jnp.arange(Q_BLOCK)
        causal = key_pos[None, :] <= qpos[:, None]
        s_idx = jax.nn.relu(jnp.einsum('bqhd,bsd->bqhs', qib, ki32))
        s_idx = jnp.einsum('bqhs,bqh->bqs', s_idx, wib)
        s_idx = jnp.where(causal[None], s_idx, -jnp.inf)
        _, sel = lax.top_k(s_idx, topk)
        valid = sel <= qpos[None, :, None]
        kg = jax.vmap(lambda kb, ib: kb[ib])(k, sel)
        vg = jax.vmap(lambda vb, ib: vb[ib])(v, sel)
        qg = qb.reshape(B, Q_BLOCK, ATT_KV_HEADS, rep, HEAD_DIM)
        s = jnp.einsum('bqgrd,bqkgd->bqgrk', qg, kg).astype(jnp.float32) * scale
        s = jnp.where(valid[:, :, None, None, :], s, -jnp.inf)
        p = jax.nn.softmax(s, axis=-1).astype(v.dtype)
        o = jnp.einsum('bqgrk,bqkgd->bqgrd', p, vg)
        return o.reshape(B, Q_BLOCK, ATT_DIM)

    out = lax.map(block, jnp.arange(n_blocks))
    return out.transpose(1, 0, 2, 3).reshape(B, L, ATT_DIM)


def hgrn2_recurrence(q, k, v, logf):
    B, L, H, DK = q.shape
    DV = v.shape[-1]
    C = HGRN_CHUNK
    nc = L // C

    def to_chunks(a):
        return a.reshape(B, nc, C, H, a.shape[-1]).transpose(1, 0, 3, 2, 4)

    tri = jnp.tril(jnp.ones((C, C), dtype=bool))

    def step(S, xs):
        qc, kc, vc, gc = xs
        b = jnp.cumsum(gc, axis=2)
        rel = jnp.where(tri[:, :, None], b[:, :, :, None, :] - b[:, :, None, :, :], -jnp.inf)
        A = jnp.einsum('bhtd,bhsd,bhtsd->bhts', qc, kc, jnp.exp(rel))
        o = jnp.einsum('bhts,bhse->bhte', A, vc) + jnp.einsum('bhtd,bhde->bhte', qc * jnp.exp(b), S)
        b_last = b[:, :, -1:, :]
        S = jnp.exp(b_last[:, :, 0, :])[..., None] * S + jnp.einsum('bhsd,bhse->bhde', kc * jnp.exp(b_last - b), vc)
        return S, o

    S0 = jnp.zeros((B, H, DK, DV), jnp.float32)
    _, o = lax.scan(step, S0, (to_chunks(q), to_chunks(k), to_chunks(v), to_chunks(logf)))
    return o.transpose(1, 0, 3, 2, 4).reshape(B, L, H, DV)


def swiglu(h, w_gate_up, w_down):
    gu = h @ w_gate_up
    gate, up = gu[..., :D_FF], gu[..., D_FF:]
    return (jax.nn.silu(gate) * up) @ w_down


def setup_inputs(seed: int = 0) -> dict:
    key = jax.random.key(seed)
    ks = jax.random.split(key, 16)
    f32 = jnp.float32

    def nrm(k, shape, fan_in):
        return jax.random.normal(k, shape, f32) * (fan_in ** -0.5)

    def gain(k, shape):
        return 1.0 + 0.02 * jax.random.normal(k, shape, f32)

    x = jax.random.normal(ks[0], (BATCH, SEQ, D_MODEL), f32)
    offset = jax.random.randint(ks[1], (BATCH, 1), 0, 4096, dtype=jnp.int32)
    positions = (offset + jnp.arange(SEQ, dtype=jnp.int32)[None, :]).astype(jnp.int32)
    return {
        "x": x,
        "positions": positions,
        "ln_mix": gain(ks[2], (DEPTH, D_MODEL)),
        "ln_ffn": gain(ks[3], (DEPTH, D_MODEL)),
        "ln_final": gain(ks[4], (D_MODEL,)),
        "w_in_ab": nrm(ks[5], (N_EVEN, D_MODEL, AB_IN), D_MODEL),
        "w_pool": nrm(ks[6], (N_EVEN, POOL_GROUPS, POOL_GROUP_DIM, POOL_GROUP_DIM), POOL_GROUP_DIM),
        "pool_scale": 1.0 + 0.1 * jax.random.normal(ks[7], (N_EVEN, POOL_DIM), f32),
        "idx_k_norm": gain(ks[8], (N_EVEN, IDX_DIM)),
        "w_out_ab": nrm(ks[9], (N_EVEN, POOL_DIM + ATT_DIM, D_MODEL), POOL_DIM + ATT_DIM),
        "w_in_c": nrm(ks[10], (N_ODD, D_MODEL, C_IN), D_MODEL),
        "hgrn_lb": 0.1 * jax.random.normal(ks[11], (DEPTH, HGRN_WIDTH), f32),
        "hgrn_out_norm": gain(ks[12], (N_ODD, HGRN_DV)),
        "w_out_c": nrm(ks[13], (N_ODD, HGRN_WIDTH, D_MODEL), HGRN_WIDTH),
        "w_gate_up": nrm(ks[14], (DEPTH, D_MODEL, 2 * D_FF), D_MODEL),
        "w_down": nrm(ks[15], (DEPTH, D_FF, D_MODEL), D_FF),
    }


def reference(x, positions, ln_mix, ln_ffn, ln_final, w_in_ab, w_pool, pool_scale, idx_k_norm,
              w_out_ab, w_in_c, hgrn_lb, hgrn_out_norm, w_out_c, w_gate_up, w_down):
    B, L = x.shape[0], x.shape[1]
    cos, sin = rope_tables(positions)
    lb_soft = jax.nn.softmax(hgrn_lb.astype(jnp.float32), axis=0)
    lb_all = jnp.cumsum(lb_soft, axis=0) - lb_soft[0:1]
    idx_w_scale = (IDX_HEADS ** -0.5) * (IDX_DIM ** -0.5)

    for layer in range(DEPTH):
        h = rmsnorm(x, ln_mix[layer])
        if layer % 2 == 0:
            e = layer // 2
            p = h @ w_in_ab[e]
            o0 = POOL_DIM
            o1 = o0 + ATT_DIM
            o2 = o1 + KV_DIM
            o3 = o2 + KV_DIM
            o4 = o3 + IDX_HEADS * IDX_DIM
            o5 = o4 + IDX_DIM
            u_pool = p[..., :o0]
            q = partial_rope(p[..., o0:o1].reshape(B, L, ATT_HEADS, HEAD_DIM), cos, sin)
            k = partial_rope(p[..., o1:o2].reshape(B, L, ATT_KV_HEADS, HEAD_DIM), cos, sin)
            v = p[..., o2:o3].reshape(B, L, ATT_KV_HEADS, HEAD_DIM)
            qi = partial_rope(p[..., o3:o4].reshape(B, L, IDX_HEADS, IDX_DIM), cos, sin)
            ki = rmsnorm(p[..., o4:o5], idx_k_norm[e])
            ki = partial_rope(ki[:, :, None, :], cos, sin)[:, :, 0, :]
            wi = p[..., o5:] * idx_w_scale
            y_a = pool_mixer(u_pool, w_pool[e], pool_scale[e])
            y_b = sparse_attention(q, k, v, qi, ki, wi)
            x = x + jnp.concatenate([y_a, y_b], axis=-1) @ w_out_ab[e]
        else:
            o = layer // 2
            p = h @ w_in_c[o]
            qz = p[..., :HGRN_WIDTH].astype(jnp.float32) * (HGRN_DK ** -0.5)
            fz = p[..., HGRN_WIDTH:2 * HGRN_WIDTH].astype(jnp.float32)
            iz = p[..., 2 * HGRN_WIDTH:3 * HGRN_WIDTH].astype(jnp.float32)
            gz = p[..., 3 * HGRN_WIDTH:]
            lb = lb_all[layer]
            f = lb + (1.0 - lb) * jax.nn.sigmoid(fz)
            kf = 1.0 - f
            shp_k = (B, L, HGRN_HEADS, HGRN_DK)
            shp_v = (B, L, HGRN_HEADS, HGRN_DV)
            oc = hgrn2_recurrence(qz.reshape(shp_k), kf.reshape(shp_k), iz.reshape(shp_v), jnp.log(f).reshape(shp_k))
            oc = rmsnorm(oc.astype(x.dtype), hgrn_out_norm[o]) * jax.nn.silu(gz).reshape(shp_v)
            x = x + oc.reshape(B, L, HGRN_WIDTH) @ w_out_c[o]
        h = rmsnorm(x, ln_ffn[layer])
        x = x + swiglu(h, w_gate_up[layer], w_down[layer])
    return rmsnorm(x, ln_final)
```

```python
import contextlib
import os
SUB = float(os.environ.get('KSUB', '99'))
import numpy as np
import concourse.bass as bass
import concourse.mybir as mybir
from concourse.bass_utils import run_bass_kernel_spmd

F32 = mybir.dt.float32
BF16 = mybir.dt.bfloat16
I32 = mybir.dt.int32
AF = mybir.ActivationFunctionType
ALU = mybir.AluOpType
AX = mybir.AxisListType

NCORES = 8
D = 1024
L_OWN = 4096
NT = 32
NKT = 64
AB_IN = 1604
DFF = 2816
TOPK = 256.0
NIT = 22
EPS = 1e-6
NEG = -1.0e30
IDX_W_SCALE = (4 ** -0.5) * (64 ** -0.5)


class Buf:
    __slots__ = ("name", "w", "r", "excl")

    def __init__(self, name, excl=False):
        self.name = name
        self.w = None
        self.r = {}
        self.excl = excl


class Src:
    def __init__(self, nc, name):
        self.name = name
        self.sem = nc.alloc_semaphore(name)
        self.val = 0


class Eng:
    def __init__(self, nc, name, handle):
        self.name = name
        self.h = handle
        self.src = Src(nc, "s_" + name)
        self.waited = {}
        self.n = 0


class FW:
    def __init__(self, nc):
        self.nc = nc
        self.pe = Eng(nc, "pe", nc.tensor)
        self.dve = Eng(nc, "dve", nc.vector)
        self.act = Eng(nc, "act", nc.scalar)
        self.pool = Eng(nc, "pool", nc.gpsimd)
        self.sp = Eng(nc, "sp", nc.sync)
        self.engs = [self.pe, self.dve, self.act, self.pool, self.sp]
        self.srcs = []

    def new_src(self, name):
        s = Src(self.nc, name)
        self.srcs.append(s)
        return s

    def _deps(self, eng, reads, writes):
        deps = {}
        for b in reads:
            ev = b.w
            if ev is not None and deps.get(ev[0], 0) < ev[1]:
                deps[ev[0]] = ev[1]
        for b in writes:
            ev = b.w
            if ev is not None and deps.get(ev[0], 0) < ev[1]:
                deps[ev[0]] = ev[1]
            for s_, v_ in b.r.items():
                if deps.get(s_, 0) < v_:
                    deps[s_] = v_
        for s, v in deps.items():
            if eng.waited.get(s, 0) < v:
                eng.h.wait_ge(s.sem, v)
                eng.waited[s] = v

    def op(self, eng, fn, reads=(), writes=()):
        ex = [b for b in reads if b.excl]
        if ex:
            reads = [b for b in reads if not b.excl]
            writes = list(writes) + ex
        self._deps(eng, reads, writes)
        ins = fn(eng.h)
        eng.src.val += 1
        ins.then_inc(eng.src.sem, 1)
        eng.n += 1
        ev = (eng.src, eng.src.val)
        for b in reads:
            b.r[ev[0]] = ev[1]
        for b in writes:
            b.w = ev
            b.r = {}
        return ins

    def dma(self, eng, dsrc, out, in_, reads=(), writes=(), **kw):
        self._deps(eng, reads, writes)
        ins = eng.h.dma_start(out=out, in_=in_, **kw)
        dsrc.val += 16
        ins.then_inc(dsrc.sem, 16)
        ev = (dsrc, dsrc.val)
        for b in reads:
            b.r[ev[0]] = ev[1]
        for b in writes:
            b.w = ev
            b.r = {}
        return ins

    def barrier(self):
        for e in self.engs:
            for s in [x.src for x in self.engs] + self.srcs:
                if s.val > 0 and e.waited.get(s, 0) < s.val:
                    e.h.wait_ge(s.sem, s.val)
                    e.waited[s] = s.val


class KB:
    def __init__(self, nc):
        self.nc = nc
        self.fw = FW(nc)
        fw = self.fw
        self.pe, self.dve, self.act, self.pool, self.sp = fw.pe, fw.dve, fw.act, fw.pool, fw.sp
        self.psall = nc.alloc_psum_tensor("psall", [128, 8, 512], F32)
        self.psb = [Buf("ps%d" % i, excl=True) for i in range(8)]
        self.wsrc = fw.new_src("wld")
        self.wsrc_sw = fw.new_src("wldsw")
        self.xsrc = [fw.new_src("xld0"), fw.new_src("xld1")]
        self.ssrc = [fw.new_src("xst0"), fw.new_src("xst1")]
        self.es = None
        self.drams = {}
        self.wpending = []

    def V(self, fn, r=(), w=()):
        return self.fw.op(self.dve, fn, r, w)

    def A(self, fn, r=(), w=()):
        return self.fw.op(self.act, fn, r, w)

    def G(self, fn, r=(), w=()):
        return self.fw.op(self.pool, fn, r, w)

    def M(self, fn, r=(), w=()):
        return self.fw.op(self.pe, fn, r, w)

    def sb(self, name, shape, dt):
        self.uid = getattr(self, "uid", 0) + 1
        t = self.es.enter_context(self.nc.sbuf_tensor("s%d_%s" % (self.uid, name), shape, dt))
        return t, Buf(name)

    def din(self, name, shape, dt=F32):
        t = self.nc.dram_tensor(name, list(shape), dt, kind="ExternalInput").ap()
        self.drams[name] = t
        return t

    def dout(self, name, shape, dt=F32):
        return self.nc.dram_tensor(name, list(shape), dt, kind="ExternalOutput").ap()

    def dint(self, name, shape, dt=F32):
        return self.nc.dram_tensor(name, list(shape), dt, kind="Internal").ap()

    def ps(self, i):
        return self.psall[:, i, :]

    def psbf(self, i):
        return self.psall[:, i, :].bitcast(BF16)

    def wload(self, dst, src, buf, cast):
        if cast:
            self.fw.dma(self.pool, self.wsrc_sw, dst, src, writes=[buf])
        else:
            self.fw.dma(self.sp, self.wsrc, dst, src, writes=[buf])
        self.wpending.append((buf, self.wsrc_sw if cast else self.wsrc))

    def wcommit(self):
        for buf, src in self.wpending:
            buf.w = (src, src.val)
        self.wpending = []

    def norm_T(self, xt_ap, xbuf, gcol, gbuf, hT_ap, hTbuf, tmp):
        (sq, sqb, st, stb, hb, hbb, bank, cst, cstb, ident, identb) = tmp
        self.A(lambda e: e.activation(out=sq[:], in_=xt_ap, func=AF.Square, scale=1.0 / 32.0, accum_out=st[:, 0:1]),
               [xbuf], [sqb, stb])
        self.G(lambda e: e.tensor_scalar(out=st[:, 1:2], in0=st[:, 0:1], scalar1=EPS, scalar2=None, op0=ALU.add), [stb], [stb])
        self.G(lambda e: e.tensor_tensor(out=st[:, 2:3], in0=st[:, 1:2], in1=cst[:, 0:1], op=ALU.pow), [stb, cstb], [stb])
        self.V(lambda e: e.tensor_scalar(out=hb[:], in0=xt_ap, scalar1=st[:, 2:3], scalar2=None, op0=ALU.mult), [xbuf, stb], [hbb])
        pb = self.psbf(bank)
        for k in range(8):
            self.M(lambda e: e.transpose(out=pb[:, k * 128:(k + 1) * 128], in_=hb[:, k * 128:(k + 1) * 128], identity=ident[:]),
                   [hbb, identb], [self.psb[bank]])
        self.V(lambda e: e.tensor_tensor(out=hT_ap, in0=pb.rearrange("p (k t) -> p k t", k=8),
                                         in1=gcol.unsqueeze(2).to_broadcast([128, 8, 128]), op=ALU.mult),
               [self.psb[bank], gbuf], [hTbuf])

    def rope(self, src3, srcbufs, dst3, dstbuf, cos2, sin2, csbuf, H, rtmp, rtmpb):
        c = cos2.unsqueeze(1).to_broadcast([128, H, 8])
        s = sin2.unsqueeze(1).to_broadcast([128, H, 8])
        x1 = src3[:, :, 0:8]
        x2 = src3[:, :, 8:16]
        t = [rtmp[:, i, 0:H, :] for i in range(4)]
        rd = list(srcbufs) + [csbuf]
        self.V(lambda e: e.tensor_tensor(out=t[0], in0=x1, in1=c, op=ALU.mult), rd, [rtmpb])
        self.V(lambda e: e.tensor_tensor(out=t[1], in0=x2, in1=s, op=ALU.mult), rd, [rtmpb])
        self.V(lambda e: e.tensor_tensor(out=t[2], in0=x2, in1=c, op=ALU.mult), rd, [rtmpb])
        self.V(lambda e: e.tensor_tensor(out=t[3], in0=x1, in1=s, op=ALU.mult), rd, [rtmpb])
        self.V(lambda e: e.tensor_tensor(out=dst3[:, :, 0:8], in0=t[0], in1=t[1], op=ALU.subtract), [rtmpb], [dstbuf])
        self.V(lambda e: e.tensor_tensor(out=dst3[:, :, 8:16], in0=t[2], in1=t[3], op=ALU.add), [rtmpb], [dstbuf])
        self.A(lambda e: e.copy(out=dst3[:, :, 16:64], in_=src3[:, :, 16:64]), list(srcbufs), [dstbuf])

    def phase_a(self, x_own, x_prev, xmid_out, ntiles=NT, nprev=NT, stop=99):
        nc = self.nc
        with contextlib.ExitStack() as es:
            self.es = es
            d = self.drams
            ident, identb = self.sb("ident", [128, 128], BF16)
            cmask, cmaskb = self.sb("cmask", [128, 128], F32)
            bands, bandsb = self.sb("bands", [128, 16, 128], BF16)
            ropeinv, ropeinvb = self.sb("ropeinv", [128, 8], F32)
            pow2, pow2b = self.sb("pow2", [128, NIT + 1], F32)
            prevbias, prevbiasb = self.sb("prevbias", [128, 1], F32)
            posi, posib = self.sb("posi", [128, NKT], I32)
            gcol, gcolb = self.sb("gcol", [128, 8], F32)
            pscale, pscaleb = self.sb("pscale", [128, 4], F32)
            gki, gkib = self.sb("gki", [128, 64], F32)
            cst, cstb = self.sb("cst", [128, 8], F32)
            self.wload(ident[:], d["ident"], identb, True)
            self.wload(cmask[:], d["cmask"], cmaskb, False)
            self.wload(bands[:], d["bands"], bandsb, True)
            self.wload(ropeinv[:], d["ropeinv"], ropeinvb, False)
            self.wload(pow2[:], d["pow2"], pow2b, False)
            self.wload(prevbias[:], d["prevbias"], prevbiasb, False)
            self.wload(posi[:], d["pos"], posib, False)
            self.wload(gcol[:], d["g_mix0"], gcolb, False)
            self.wload(pscale[:], d["pscale"], pscaleb, False)
            self.wload(gki[:], d["idx_k_norm"].rearrange("(o n) -> o n", o=1).to_broadcast([128, 64]), gkib, False)
            win, winb = self.sb("win", [128, 8, 1664], BF16)
            self.G(lambda e: e.memset(win[:, :, 1600:1664], 0.0), [], [winb])
            wouta, woutab = self.sb("wouta", [128, 4, 1024], BF16)
            woutb, woutbb = self.sb("woutb", [64, 8, 1024], BF16)
            wpool, wpoolb = self.sb("wpool", [128, 4, 128], BF16)
            w_in = d["w_in_ab"].rearrange("(k p) n -> p k n", p=128)
            for k in range(8):
                self.wload(win[:, k, 0:AB_IN], w_in[:, k, :], winb, True)
            wo = d["w_out_ab"]
            self.wload(wouta[:], wo[0:512, :].rearrange("(k p) n -> p k n", p=128), woutab, True)
            self.wload(woutb[:], wo[512:1024, :].rearrange("(h p) n -> p h n", p=64), woutbb, True)
            self.wload(wpool[:], d["w_pool"].rearrange("g c d -> c g d"), wpoolb, True)
            self.wcommit()
            KK, _ = self.sb("KK", [128, NKT * 128], BF16)
            kiT, _ = self.sb("kiT", [128, NKT * 128], BF16)
            Vaug, _ = self.sb("Vaug", [128, NKT, 2, 65], BF16)
            keyb = [Buf("key%d" % i) for i in range(NKT)]
            acc, accb = self.sb("acc", [128, NKT * 128], F32)
            xt = []
            for i in range(2):
                xt.append(self.sb("xt%d" % i, [128, 1024], F32))
            sq, sqb = self.sb("sq", [128, 1024], BF16)
            st, stb = self.sb("st", [128, 8], F32)
            hb, hbb = self.sb("hb", [128, 1024], BF16)
            hT, hTb = self.sb("hT", [128, 8, 128], BF16)
            cosT, cosTb = self.sb("cosT", [128, NKT, 8], F32)
            sinT, sinTb = self.sb("sinT", [128, NKT, 8], F32)
            rwork, rworkb = self.sb("rwork", [128, NKT * 8], F32)
            rwork2, rwork2b = self.sb("rwork2", [128, NKT * 8], F32)
            rworki, rworkib = self.sb("rworki", [128, NKT * 8], I32)
            rtmp, rtmpb = self.sb("rtmp", [128, 4, 8, 8], F32)
            U = [self.sb("U%d" % i, [128, 512], BF16) for i in range(2)]
            pooledT, pooledTb = self.sb("pooledT", [128, 4, 128], BF16)
            yaT, yaTb = self.sb("yaT", [128, 4, 128], BF16)
            q_r, q_rb = self.sb("q_r", [128, 4, 2, 64], BF16)
            qsb, qsbb = self.sb("qsb", [128, 512], F32)
            k_r, k_rb = self.sb("k_r", [128, 2, 64], BF16)
            qi_r, qi_rb = self.sb("qi_r", [128, 4, 64], BF16)
            kin, kinb = self.sb("kin", [128, 1, 64], F32)
            ki_r, ki_rb = self.sb("ki_r", [128, 2, 64], BF16)
            qT, qTb = self.sb("qT", [128, 4, 128], BF16)
            qiT, qiTb = self.sb("qiT", [128, 2, 128], BF16)
            wcol, wcolb = self.sb("wcol", [128, 8], F32)
            bt, btb = self.sb("bt", [128, 16], F32)
            hcols, hcolsb = self.sb("hcols", [128, NIT + 1], F32)
            qsq, qsqb = self.sb("qsq", [128, 256], F32)
            bis, bisb = self.sb("bis", [128, 8], F32)
            junk, junkb = self.sb("junk", [128, 2], BF16)
            relu = [self.sb("relu%d" % i, [128, 512], F32) for i in range(2)]
            mk = [self.sb("mk%d" % i, [128, 128], BF16) for i in range(4)]
            E = [self.sb("E%d" % i, [128, 1024], BF16) for i in range(2)]
            rec, recb = self.sb("rec", [128, 1024], F32)
            bcs, bcsb = self.sb("bcs", [64, 1024], F32)
            ones, onesb = self.sb("ones", [128, 64], F32)
            ybT, ybTb = self.sb("ybT", [64, 8, 128], BF16)
            psb = self.psb
            ntmp = (sq, sqb, st, stb, hb, hbb, 4, cst, cstb, ident, identb)

            self.G(lambda e: e.memset(cst[:, 0:1], -0.5), [], [cstb])
            self.G(lambda e: e.memset(cst[:, 4:8], 0.5), [], [cstb])
            self.G(lambda e: e.memset(ones[:], 1.0), [], [onesb])
            self.G(lambda e: e.memset(Vaug[:], 1.0), [], keyb)
            self.V(lambda e: e.tensor_reduce(out=cst[:, 3:4], in_=gki[:], axis=AX.X, op=ALU.max, apply_absolute_value=True), [gkib], [cstb])
            self.V(lambda e: e.tensor_scalar(out=cst[:, 2:3], in0=cst[:, 3:4], scalar1=8.0 * 1.01, scalar2=None, op0=ALU.mult), [cstb], [cstb])
            posf = rwork2
            self.V(lambda e: e.tensor_copy(out=posf[:, 0:NKT], in_=posi[:]), [posib], [rwork2b])
            ang, angb = self.sb("ang", [128, NKT, 8], F32)
            self.V(lambda e: e.tensor_tensor(out=ang[:], in0=posf[:, 0:NKT].unsqueeze(2).to_broadcast([128, NKT, 8]),
                                             in1=ropeinv[:].unsqueeze(1).to_broadcast([128, NKT, 8]), op=ALU.mult), [rwork2b, ropeinvb], [angb])
            angf = ang[:].rearrange("p a b -> p (a b)")
            for (tab, tabb, shift) in ((sinT, sinTb, 0.0), (cosT, cosTb, float(np.pi / 2))):
                tabf = tab[:].rearrange("p a b -> p (a b)")
                self.V(lambda e: e.tensor_scalar(out=rwork[:], in0=angf, scalar1=shift, scalar2=float(1.0 / (2 * np.pi)), op0=ALU.add, op1=ALU.mult), [angb], [rworkb])
                self.V(lambda e: e.tensor_copy(out=rworki[:], in_=rwork[:]), [rworkb], [rworkib])
                self.V(lambda e: e.tensor_copy(out=rwork[:], in_=rworki[:]), [rworkib], [rworkb])
                self.V(lambda e: e.scalar_tensor_tensor(out=rwork[:], in0=rwork[:], scalar=float(-2 * np.pi), in1=angf, op0=ALU.mult, op1=ALU.add), [rworkb, angb], [rworkb])
                if shift != 0.0:
                    self.V(lambda e: e.tensor_scalar(out=rwork[:], in0=rwork[:], scalar1=shift, scalar2=None, op0=ALU.add), [rworkb], [rworkb])
                self.V(lambda e: e.tensor_scalar(out=rwork2[:], in0=rwork[:], scalar1=float(np.pi), scalar2=float(-2 * np.pi), op0=ALU.is_gt, op1=ALU.mult), [rworkb], [rwork2b])
                self.V(lambda e: e.tensor_tensor(out=rwork[:], in0=rwork[:], in1=rwork2[:], op=ALU.add), [rworkb, rwork2b], [rworkb])
                self.V(lambda e: e.tensor_scalar(out=rwork2[:], in0=rwork[:], scalar1=float(-np.pi), scalar2=float(2 * np.pi), op0=ALU.is_lt, op1=ALU.mult), [rworkb], [rwork2b])
                self.V(lambda e: e.tensor_tensor(out=rwork[:], in0=rwork[:], in1=rwork2[:], op=ALU.add), [rworkb, rwork2b], [rworkb])
                self.V(lambda e: e.tensor_scalar(out=rwork[:], in0=rwork[:], scalar1=float(-np.pi), scalar2=float(np.pi), op0=ALU.max, op1=ALU.min), [rworkb], [rworkb])
                self.A(lambda e: e.activation(out=tabf, in_=rwork[:], func=AF.Sin), [rworkb], [tabb])

            def load_x(slot, src_ap):
                self.fw.dma(self.sp, self.xsrc[slot], xt[slot][0][:], src_ap, writes=[xt[slot][1]])

            def key_post(kt, kbank, kcol, vcol, kibank, kicol):
                pk = self.ps(kbank)
                pki = self.ps(kibank)
                cos2 = cosT[:, kt, :]
                sin2 = sinT[:, kt, :]
                self.rope(pk[:, kcol:kcol + 128].rearrange("p (h d) -> p h d", h=2), [psb[kbank]], k_r[:], k_rb, cos2, sin2, cosTb, 2, rtmp, rtmpb)
                self.A(lambda e: e.copy(out=Vaug[:, kt, :, 0:64], in_=pk[:, vcol:vcol + 128].rearrange("p (g d) -> p g d", g=2)), [psb[kbank]], [keyb[kt]])
                if SUB == 2.3:
                    return
                self.A(lambda e: e.activation(out=sq[:, 0:64], in_=pki[:, kicol:kicol + 64], func=AF.Square, scale=0.125, accum_out=st[:, 4:5]), [psb[kibank]], [sqb, stb])
                self.G(lambda e: e.tensor_scalar(out=st[:, 5:6], in0=st[:, 4:5], scalar1=EPS, scalar2=None, op0=ALU.add), [stb], [stb])
                self.G(lambda e: e.tensor_tensor(out=st[:, 6:7], in0=st[:, 5:6], in1=cst[:, 0:1], op=ALU.pow), [stb, cstb], [stb])
                self.V(lambda e: e.scalar_tensor_tensor(out=kin[:, 0, :], in0=pki[:, kicol:kicol + 64], scalar=st[:, 6:7], in1=gki[:], op0=ALU.mult, op1=ALU.mult), [psb[kibank], stb, gkib], [kinb])
                self.rope(kin[:], [kinb], ki_r[:, 0:1, :], ki_rb, cos2, sin2, cosTb, 1, rtmp, rtmpb)
                self.V(lambda e: e.tensor_copy(out=ki_r[:, 1, :], in_=ki_r[:, 0, :]), [ki_rb], [ki_rb])
                if SUB == 2.6:
                    return
                pb5 = self.psbf(5)
                self.M(lambda e: e.transpose(out=pb5[:, 512:640], in_=k_r[:].rearrange("p h d -> p (h d)"), identity=ident[:]), [k_rb, identb], [psb[5]])
                self.M(lambda e: e.transpose(out=pb5[:, 896:1024], in_=ki_r[:].rearrange("p h d -> p (h d)"), identity=ident[:]), [ki_rb, identb], [psb[5]])
                self.A(lambda e: e.copy(out=KK[:, kt * 128:(kt + 1) * 128], in_=pb5[:, 512:640]), [psb[5]], [keyb[kt]])
                self.A(lambda e: e.copy(out=kiT[:, kt * 128:(kt + 1) * 128], in_=pb5[:, 896:1024]), [psb[5]], [keyb[kt]])

            if stop <= 1:
                self.fw.barrier()
                return
            if nprev > 0:
                load_x(0, x_prev[0:128, :])
            for kt in range(nprev):
                slot = kt % 2
                if kt + 1 < nprev:
                    load_x(1 - slot, x_prev[(kt + 1) * 128:(kt + 2) * 128, :])
                else:
                    load_x(1 - slot, x_own[0:128, :])
                xtile, xb_ = xt[slot]
                self.norm_T(xtile[:], xb_, gcol[:], gcolb, hT[:], hTb, ntmp)
                p0 = self.ps(0)
                for k in range(8):
                    self.M(lambda e: e.matmul(p0[:, 0:256], lhsT=hT[:, k, :], rhs=win[:, k, 1024:1280], start=(k == 0), stop=(k == 7)), [hTb, winb], [psb[0]])
                for k in range(8):
                    self.M(lambda e: e.matmul(p0[:, 256:320], lhsT=hT[:, k, :], rhs=win[:, k, 1536:1600], start=(k == 0), stop=(k == 7)), [hTb, winb], [psb[0]])
                if kt == nprev - 1:
                    p1 = self.ps(1)
                    for k in range(8):
                        self.M(lambda e: e.matmul(p1[:], lhsT=hT[:, k, :], rhs=win[:, k, 0:512], start=(k == 0), stop=(k == 7)), [hTb, winb], [psb[1]])
                    self.A(lambda e: e.copy(out=U[1][0][:], in_=p1[:]), [psb[1]], [U[1][1]])
                key_post(kt, 0, 0, 128, 0, 256)
            if nprev == 0:
                load_x(0, x_own[0:128, :])
                self.G(lambda e: e.memset(U[1][0][:], 0.0), [], [U[1][1]])

            if stop <= 2:
                self.fw.barrier()
                return
            for j in range(ntiles):
                kt = nprev + j
                slot = (nprev + j) % 2
                xtile, xb_ = xt[slot]
                self.norm_T(xtile[:], xb_, gcol[:], gcolb, hT[:], hTb, ntmp)
                for b in range(int(os.environ.get('KNB', '4'))):
                    c0, c1 = b * 512, min((b + 1) * 512, 1664)
                    pbk = self.ps(b)
                    for k in range(8):
                        self.M(lambda e: e.matmul(pbk[:, 0:c1 - c0], lhsT=hT[:, k, :], rhs=win[:, k, c0:c1], start=(k == 0), stop=(k == 7)), [hTb, winb], [psb[b]])
                if SUB <= 1:
                    continue
                Uc, Ucb = U[j % 2]
                Up, Upb = U[1 - j % 2]
                self.A(lambda e: e.copy(out=Uc[:], in_=self.ps(0)), [psb[0]], [Ucb])
                if SUB <= 1.3:
                    continue
                cos2 = cosT[:, kt, :]
                sin2 = sinT[:, kt, :]
                self.A(lambda e: e.copy(out=qsb[:], in_=self.ps(1)), [psb[1]], [qsbb])
                for g in range((2 if SUB > 1.5 else 1) if not os.environ.get('KSKIPQ') else 0):
                    self.rope(qsb[:, g * 256:(g + 1) * 256].rearrange("p (h d) -> p h d", h=4), [qsbb], q_r[:, :, g, :], q_rb, cos2, sin2, cosTb, 4, rtmp, rtmpb)
                if SUB <= 1.6:
                    continue
                if not os.environ.get('KSKIPQI'):
                  self.rope(self.ps(2)[:, 256:512].rearrange("p (h d) -> p h d", h=4), [psb[2]], qi_r[:], qi_rb, cos2, sin2, cosTb, 4, rtmp, rtmpb)
                if SUB <= 2:
                    continue
                key_post(kt, 2, 0, 128, 3, 0)
                if SUB <= 3:
                    continue
                self.V(lambda e: e.tensor_scalar(out=wcol[:, 0:4], in0=self.ps(3)[:, 64:68], scalar1=IDX_W_SCALE, scalar2=None, op0=ALU.mult), [psb[3]], [wcolb])
                self.V(lambda e: e.tensor_tensor(out=qsq[:], in0=qi_r[:].rearrange("p h d -> p (h d)"), in1=qi_r[:].rearrange("p h d -> p (h d)"), op=ALU.mult), [qi_rb], [qsqb])
                self.V(lambda e: e.tensor_reduce(out=bt[:, 0:4], in_=qsq[:].rearrange("p (h d) -> p h d", h=4), axis=AX.X, op=ALU.add), [qsqb], [btb])
                self.G(lambda e: e.tensor_tensor(out=bt[:, 4:8], in0=bt[:, 0:4], in1=cst[:, 4:8], op=ALU.pow), [btb, cstb], [btb])
                self.V(lambda e: e.tensor_scalar(out=wcol[:, 4:8], in0=wcol[:, 0:4], scalar1=-1.0, scalar2=None, op0=ALU.mult), [wcolb], [wcolb])
                self.V(lambda e: e.tensor_tensor(out=bt[:, 8:12], in0=wcol[:, 0:4], in1=wcol[:, 4:8], op=ALU.max), [wcolb], [btb])
                self.V(lambda e: e.tensor_tensor(out=bt[:, 8:12], in0=bt[:, 8:12], in1=bt[:, 4:8], op=ALU.mult), [btb], [btb])
                self.V(lambda e: e.tensor_reduce(out=bt[:, 12:13], in_=bt[:, 8:12], axis=AX.X, op=ALU.add), [btb], [btb])
                self.V(lambda e: e.tensor_scalar(out=bt[:, 13:14], in0=bt[:, 12:13], scalar1=cst[:, 2:3], scalar2=None, op0=ALU.mult), [btb, cstb], [btb])
                self.V(lambda e: e.tensor_scalar(out=hcols[:], in0=pow2[:], scalar1=bt[:, 13:14], scalar2=None, op0=ALU.mult), [pow2b, btb], [hcolsb])
                if SUB <= 4:
                    continue
                pb5 = self.psbf(5)
                for p in range(4):
                    self.M(lambda e: e.transpose(out=pb5[:, p * 128:(p + 1) * 128], in_=q_r[:, p, :, :].rearrange("p g d -> p (g d)"), identity=ident[:]), [q_rb, identb], [psb[5]])
                for p in range(2):
                    self.M(lambda e: e.transpose(out=pb5[:, 640 + p * 128:640 + (p + 1) * 128], in_=qi_r[:, 2 * p:2 * p + 2, :].rearrange("p h d -> p (h d)"), identity=ident[:]), [qi_rb, identb], [psb[5]])
                self.A(lambda e: e.copy(out=qT[:].rearrange("p a t -> p (a t)"), in_=pb5[:, 0:512]), [psb[5]], [qTb])
                self.A(lambda e: e.copy(out=qiT[:].rearrange("p a t -> p (a t)"), in_=pb5[:, 640:896]), [psb[5]], [qiTb])
                if j + 1 < ntiles:
                    load_x(1 - slot, x_own[(j + 1) * 128:(j + 2) * 128, :])
                if stop <= 3:
                    continue
                first = (j == 0)
                p6 = self.ps(6)
                for g in range(4):
                    bc_i = (8 + g) if first else g
                    bp_i = (12 + g) if first else (4 + g)
                    self.M(lambda e: e.matmul(p6[:, g * 128:(g + 1) * 128], lhsT=Uc[:, g * 128:(g + 1) * 128], rhs=bands[:, bc_i, :], start=True, stop=False), [Ucb, bandsb], [psb[6]])
                    self.M(lambda e: e.matmul(p6[:, g * 128:(g + 1) * 128], lhsT=Up[:, g * 128:(g + 1) * 128], rhs=bands[:, bp_i, :], start=False, stop=True), [Upb, bandsb], [psb[6]])
                self.A(lambda e: e.copy(out=pooledT[:].rearrange("p g t -> p (g t)"), in_=p6[:]), [psb[6]], [pooledTb])
                p7 = self.ps(7)
                for g in range(4):
                    self.M(lambda e: e.matmul(p7[:, g * 128:(g + 1) * 128], lhsT=wpool[:, g, :], rhs=pooledT[:, g, :], start=True, stop=True), [wpoolb, pooledTb], [psb[7]])
                self.V(lambda e: e.tensor_tensor(out=yaT[:], in0=p7.rearrange("p (g t) -> p g t", g=4), in1=pscale[:].unsqueeze(2).to_broadcast([128, 4, 128]), op=ALU.mult), [psb[7], pscaleb], [yaTb])
                if stop <= 4:
                    continue
                nk = (kt + 1) * 128
                nch = (nk + 511) // 512
                ib = 0
                for c in range(nch):
                    s0 = c * 512
                    n = min(512, nk - s0)
                    kbufs = keyb[s0 // 128:(s0 + n) // 128]
                    for h in range(4):
                        bank = ib % 2
                        ib += 1
                        r0 = (h % 2) * 64
                        pI = self.ps(bank)
                        self.M(lambda e: e.matmul(pI[:, 0:n], lhsT=qiT[r0:r0 + 64, h // 2, :], rhs=kiT[r0:r0 + 64, s0:s0 + n], start=True, stop=True), [qiTb] + kbufs, [psb[bank]])
                        rl, rlb = relu[h % 2]
                        self.A(lambda e: e.activation(out=rl[:, 0:n], in_=pI[:, 0:n], func=AF.Relu), [psb[bank]], [rlb])
                        if h == 0:
                            self.V(lambda e: e.tensor_scalar(out=acc[:, s0:s0 + n], in0=rl[:, 0:n], scalar1=wcol[:, 0:1], scalar2=None, op0=ALU.mult), [rlb, wcolb], [accb])
                        else:
                            self.V(lambda e: e.scalar_tensor_tensor(out=acc[:, s0:s0 + n], in0=rl[:, 0:n], scalar=wcol[:, h:h + 1], in1=acc[:, s0:s0 + n], op0=ALU.mult, op1=ALU.add), [rlb, wcolb, accb], [accb])
                self.V(lambda e: e.tensor_tensor(out=acc[:, nk - 128:nk], in0=acc[:, nk - 128:nk], in1=cmask[:], op=ALU.add), [accb, cmaskb], [accb])
                if nprev > 0:
                    self.V(lambda e: e.tensor_scalar(out=acc[:, 0:nprev * 128], in0=acc[:, 0:nprev * 128], scalar1=prevbias[:, 0:1], scalar2=None, op0=ALU.add), [accb, prevbiasb], [accb])
                self.V(lambda e: e.memset(bis[:, 0:1], 0.0), [], [bisb])
                for i in range(NIT):
                    self.V(lambda e: e.tensor_scalar(out=junk[:, 0:1].to_broadcast([128, nk]), in0=acc[:, 0:nk], scalar1=bis[:, 0:1], scalar2=None, op0=ALU.is_ge, op1=ALU.add, accum_out=bis[:, 1:2]), [accb, bisb], [junkb, bisb])
                    self.V(lambda e: e.tensor_tensor(out=bis[:, 2:3], in0=bis[:, 0:1], in1=hcols[:, i + 1:i + 2], op=ALU.subtract), [bisb, hcolsb], [bisb])
                    self.V(lambda e: e.tensor_scalar(out=bis[:, 3:4], in0=bis[:, 1:2], scalar1=TOPK, scalar2=None, op0=ALU.is_ge), [bisb], [bisb])
                    self.V(lambda e: e.scalar_tensor_tensor(out=bis[:, 0:1], in0=bis[:, 3:4], scalar=hcols[:, i:i + 1], in1=bis[:, 2:3], op0=ALU.mult, op1=ALU.add), [bisb, hcolsb], [bisb])
                self.V(lambda e: e.tensor_tensor(out=bis[:, 4:5], in0=bis[:, 0:1], in1=hcols[:, NIT:NIT + 1], op=ALU.subtract), [bisb, hcolsb], [bisb])
                if stop <= 5:
                    continue
                pbm = [self.psbf(4), self.psbf(5)]
                nkc = kt + 1
                for kc in range(nkc):
                    mt, mtb = mk[kc % 4]
                    self.V(lambda e: e.tensor_scalar(out=mt[:], in0=acc[:, kc * 128:(kc + 1) * 128], scalar1=bis[:, 4:5], scalar2=None, op0=ALU.is_ge), [accb, bisb], [mtb])
                    mslot = kc % 2
                    self.M(lambda e: e.transpose(out=pbm[mslot][:, 0:128], in_=mt[:], identity=ident[:]), [mtb, identb], [psb[4 + mslot]])
                    sb0 = 2 * (kc % 2)
                    for g in range(2):
                        self.M(lambda e: e.matmul(self.ps(sb0 + g), lhsT=KK[g * 64:(g + 1) * 64, kc * 128:(kc + 1) * 128], rhs=qT[g * 64:(g + 1) * 64, :, :].rearrange("p a t -> p (a t)"), start=True, stop=True), [keyb[kc], qTb], [psb[sb0 + g]])
                    Et, Etb = E[kc % 2]
                    self.A(lambda e: e.activation(out=Et[:], in_=self.psall[:, sb0:sb0 + 2, :].rearrange("p a n -> p (a n)"), func=AF.Exp, scale=0.125), [psb[sb0], psb[sb0 + 1]], [Etb])
                    self.V(lambda e: e.tensor_tensor(out=Et[:].rearrange("p (a t) -> p a t", a=8), in0=Et[:].rearrange("p (a t) -> p a t", a=8),
                                                     in1=pbm[mslot][:, 0:128].unsqueeze(1).to_broadcast([128, 8, 128]), op=ALU.mult), [Etb, psb[4 + mslot]], [Etb])
                    for g in range(2):
                        self.M(lambda e: e.matmul(self.ps(6 + g)[0:65, :], lhsT=Vaug[:, kc, g, :], rhs=Et[:, g * 512:(g + 1) * 512], start=(kc == 0), stop=(kc == nkc - 1)), [keyb[kc], Etb], [psb[6 + g]])
                o2 = self.psall[:, 6:8, :].rearrange("p a n -> p (a n)")
                self.V(lambda e: e.reciprocal(out=rec[64:65, :], in_=o2[64:65, :]), [psb[6], psb[7]], [recb])
                for g in range(2):
                    self.M(lambda e: e.matmul(self.ps(g)[0:64, :], lhsT=ones[64:65, 0:64], rhs=rec[64:65, g * 512:(g + 1) * 512], start=True, stop=True), [onesb, recb], [psb[g]])
                self.A(lambda e: e.copy(out=bcs[:], in_=self.psall[0:64, 0:2, :].rearrange("p a n -> p (a n)")), [psb[0], psb[1]], [bcsb])
                self.V(lambda e: e.tensor_tensor(out=ybT[:].rearrange("p h t -> p (h t)"), in0=o2[0:64, :], in1=bcs[:], op=ALU.mult), [psb[6], psb[7], bcsb], [ybTb])
                if stop <= 6:
                    continue
                for n in range(2):
                    po = self.ps(2 + n)
                    for g in range(4):
                        self.M(lambda e: e.matmul(po, lhsT=yaT[:, g, :], rhs=wouta[:, g, n * 512:(n + 1) * 512], start=(g == 0), stop=False), [yaTb, woutab], [psb[2 + n]])
                    for h in range(8):
                        self.M(lambda e: e.matmul(po, lhsT=ybT[:, h, :], rhs=woutb[:, h, n * 512:(n + 1) * 512], start=False, stop=(h == 7)), [ybTb, woutbb], [psb[2 + n]])
                self.V(lambda e: e.tensor_tensor(out=xtile[:], in0=xtile[:], in1=self.psall[:, 2:4, :].rearrange("p a n -> p (a n)"), op=ALU.add), [xb_, psb[2], psb[3]], [xb_])
                self.fw.dma(self.sp, self.ssrc[slot], xmid_out[j * 128:(j + 1) * 128, :], xtile[:], reads=[xb_])
            self.fw.barrier()
        self.es = None


    def phase_ffn(self, xin, xout, sfx, final=False, ntiles=NT):
        with contextlib.ExitStack() as es:
            self.es = es
            d = self.drams
            ident, identb = self.sb("ident", [128, 128], BF16)
            gcol, gcolb = self.sb("gcol", [128, 8], F32)
            cst, cstb = self.sb("cst", [128, 8], F32)
            wgu, wgub = self.sb("wgu", [128, 8, 2 * DFF], BF16)
            wd, wdb = self.sb("wd", [128, 22, 1024], BF16)
            self.wload(ident[:], d["ident"], identb, True)
            self.wload(gcol[:], d["g_ffn" + sfx], gcolb, False)
            wg = d["w_gate_up" + sfx].rearrange("(k p) n -> p k n", p=128)
            for k in range(8):
                self.wload(wgu[:, k, :], wg[:, k, :], wgub, True)
            wdd = d["w_down" + sfx].rearrange("(k p) n -> p k n", p=128)
            for k0 in range(0, 22, 6):
                k1 = min(22, k0 + 6)
                self.wload(wd[:, k0:k1, :], wdd[:, k0:k1, :], wdb, True)
            if final:
                gfin, gfinb = self.sb("gfin", [128, 1024], F32)
                self.wload(gfin[:], d["ln_final"].rearrange("(o n) -> o n", o=1).to_broadcast([128, 1024]), gfinb, False)
            self.wcommit()
            xt = [self.sb("xf%d" % i, [128, 2, 1024], F32) for i in range(2)]
            sq, sqb = self.sb("sq", [128, 1024], BF16)
            st, stb = self.sb("st", [128, 8], F32)
            hb, hbb = self.sb("hb", [128, 1024], BF16)
            hT, hTb = self.sb("hT", [128, 8, 256], BF16)
            aT, aTb = self.sb("aT", [128, 22, 256], BF16)
            sg = [self.sb("sg%d" % i, [128, 256], F32) for i in range(2)]
            psb = self.psb
            ntmp = (sq, sqb, st, stb, hb, hbb, 4, cst, cstb, ident, identb)
            self.G(lambda e: e.memset(cst[:, 0:1], -0.5), [], [cstb])
            nm = ntiles // 2

            def load(m):
                slot = m % 2
                self.fw.dma(self.sp, self.xsrc[slot], xt[slot][0][:], xin[m * 256:(m + 1) * 256, :].rearrange("(s p) n -> p s n", p=128), writes=[xt[slot][1]])
            load(0)
            for m in range(nm):
                slot = m % 2
                xm, xmb = xt[slot]
                if m + 1 < nm:
                    load(m + 1)
                for sub in range(2):
                    self.norm_T(xm[:, sub, :], xmb, gcol[:], gcolb, hT[:, :, sub * 128:(sub + 1) * 128], hTb, ntmp)
                for fc in range(22):
                    bank = fc % 4
                    pg = self.ps(bank)
                    for half in range(2):
                        c0 = half * DFF + fc * 128
                        for k in range(8):
                            self.M(lambda e: e.matmul(pg[:, half * 256:(half + 1) * 256], lhsT=wgu[:, k, c0:c0 + 128], rhs=hT[:, k, :], start=(k == 0), stop=(k == 7)), [wgub, hTb], [psb[bank]])
                    sgt, sgb = sg[fc % 2]
                    self.A(lambda e: e.activation(out=sgt[:], in_=pg[:, 0:256], func=AF.Silu), [psb[bank]], [sgb])
                    self.V(lambda e: e.tensor_tensor(out=aT[:, fc, :], in0=sgt[:], in1=pg[:, 256:512], op=ALU.mult), [sgb, psb[bank]], [aTb])
                for sub in range(2):
                    for n in range(2):
                        po = self.ps(5 + n)
                        for fc in range(22):
                            self.M(lambda e: e.matmul(po, lhsT=aT[:, fc, sub * 128:(sub + 1) * 128], rhs=wd[:, fc, n * 512:(n + 1) * 512], start=(fc == 0), stop=(fc == 21)), [aTb, wdb], [psb[5 + n]])
                    self.V(lambda e: e.tensor_tensor(out=xm[:, sub, :], in0=xm[:, sub, :], in1=self.psall[:, 5:7, :].rearrange("p a n -> p (a n)"), op=ALU.add), [xmb, psb[5], psb[6]], [xmb])
                    if final:
                        self.A(lambda e: e.activation(out=sq[:], in_=xm[:, sub, :], func=AF.Square, scale=1.0 / 32.0, accum_out=st[:, 4:5]), [xmb], [sqb, stb])
                        self.G(lambda e: e.tensor_scalar(out=st[:, 5:6], in0=st[:, 4:5], scalar1=EPS, scalar2=None, op0=ALU.add), [stb], [stb])
                        self.G(lambda e: e.tensor_tensor(out=st[:, 6:7], in0=st[:, 5:6], in1=cst[:, 0:1], op=ALU.pow), [stb, cstb], [stb])
                        self.V(lambda e: e.scalar_tensor_tensor(out=xm[:, sub, :], in0=xm[:, sub, :], scalar=st[:, 6:7], in1=gfin[:], op0=ALU.mult, op1=ALU.mult), [xmb, stb, gfinb], [xmb])
                self.fw.dma(self.sp, self.ssrc[slot], xout[m * 256:(m + 1) * 256, :].rearrange("(s p) n -> p s n", p=128), xm[:], reads=[xmb])
            self.fw.barrier()
        self.es = None

    def phase_hgrn(self, xin, xout, sinit, send, state_only, ntiles=NT):
        with contextlib.ExitStack() as es:
            self.es = es
            d = self.drams
            ident, identb = self.sb("ident", [128, 128], BF16)
            gcol, gcolb = self.sb("gcol", [128, 8], F32)
            cst, cstb = self.sb("cst", [128, 16], F32)
            tri, trib = self.sb("tri", [128, 128], F32)
            triu, triub = self.sb("triu", [128, 128], F32)
            cind, cindb = self.sb("cind", [128, 64], F32)
            LB, LBb = self.sb("LB", [128, 1024], F32)
            OML, OMLb = self.sb("OML", [128, 1024], F32)
            win, winb = self.sb("winc", [128, 8, 4096], BF16)
            S, Sb_ = self.sb("S", [128, 8, 128], F32)
            Sh, Shb = self.sb("Sh", [128, 8, 128], BF16)
            Shm, Shmb = self.sb("Shm", [128, 8, 128], BF16)
            self.wload(ident[:], d["ident"], identb, True)
            self.wload(gcol[:], d["g_mix1"], gcolb, False)
            self.wload(tri[:], d["tribd"], trib, False)
            self.wload(triu[:], d["triu"], triub, False)
            self.wload(cind[:], d["cind"], cindb, False)
            self.wload(LB[:], d["hgrn_lb"][1:2, :].to_broadcast([128, 1024]), LBb, False)
            self.wload(OML[:], d["hgrn_lb"][0:1, :].to_broadcast([128, 1024]), OMLb, False)
            self.wload(S[:], sinit, Sb_, False)
            wi_ = d["w_in_c"].rearrange("(k p) n -> p k n", p=128)
            for k in range(8):
                self.wload(win[:, k, :], wi_[:, k, :], winb, True)
            if not state_only:
                woc, wocb = self.sb("woc", [128, 8, 1024], BF16)
                NG, NGb = self.sb("NG", [128, 128], F32)
                self.wload(woc[:], d["w_out_c"].rearrange("(k p) n -> p k n", p=128), wocb, True)
                self.wload(NG[:], d["hgrn_out_norm"].rearrange("(o n) -> o n", o=1).to_broadcast([128, 128]), NGb, False)
            self.wcommit()
            xt = [self.sb("xh%d" % i, [128, 1024], F32) for i in range(2)]
            sq, sqb = self.sb("sq", [128, 1024], BF16)
            st, stb = self.sb("st", [128, 32], F32)
            hb, hbb = self.sb("hb", [128, 1024], BF16)
            hT, hTb = self.sb("hT", [128, 8, 128], BF16)
            fS, fSb = self.sb("fS", [128, 1024], F32)
            lg, lgb = self.sb("lg", [128, 1024], F32)
            kf, kfb = self.sb("kf", [128, 1024], F32)
            e3, e3b = self.sb("e3", [128, 1024], F32)
            v, vb = self.sb("v", [128, 1024], BF16)
            khat, khatb = self.sb("khat", [128, 1024], BF16)
            dec, decb = self.sb("dec", [128, 8, 64], F32)
            if not state_only:
                e1, e1b = self.sb("e1", [128, 1024], F32)
                e2, e2b = self.sb("e2", [128, 1024], F32)
                gs, gsb = self.sb("gs", [128, 1024], F32)
                qt, qtb = self.sb("qt", [128, 1024], BF16)
                ktl, ktlb = self.sb("ktl", [128, 1024], BF16)
                ocb, ocbb = self.sb("ocb", [128, 1024], BF16)
                qT, qTb = self.sb("qT", [128, 8, 128], BF16)
                kT, kTb = self.sb("kT", [128, 8, 128], BF16)
                ocT, ocTb = self.sb("ocT", [128, 8, 128], BF16)
                Am = [self.sb("Am%d" % i, [128, 128], BF16) for i in range(2)]
            psb = self.psb
            ntmp = (sq, sqb, st, stb, hb, hbb, 4, cst, cstb, ident, identb)
            self.G(lambda e: e.memset(cst[:, 0:1], -0.5), [], [cstb])
            self.G(lambda e: e.memset(cst[:, 8:16], -0.5), [], [cstb])
            self.V(lambda e: e.tensor_tensor(out=LB[:], in0=LB[:], in1=OML[:], op=ALU.subtract), [LBb, OMLb], [LBb])
            self.A(lambda e: e.activation(out=LB[:], in_=LB[:], func=AF.Sigmoid), [LBb], [LBb])
            self.V(lambda e: e.tensor_scalar(out=OML[:], in0=LB[:], scalar1=-1.0, scalar2=1.0, op0=ALU.mult, op1=ALU.add), [LBb], [OMLb])
            self.A(lambda e: e.copy(out=Sh[:], in_=S[:]), [Sb_], [Shb])
            p01 = self.psall[:, 0:2, :].rearrange("p a n -> p (a n)")
            p23 = self.psall[:, 2:4, :].rearrange("p a n -> p (a n)")
            p56 = self.psall[:, 5:7, :].rearrange("p a n -> p (a n)")

            def proj(col0, b0):
                for n in range(2):
                    for k in range(8):
                        self.M(lambda e: e.matmul(self.ps(b0 + n), lhsT=hT[:, k, :], rhs=win[:, k, col0 + n * 512:col0 + (n + 1) * 512], start=(k == 0), stop=(k == 7)), [hTb, winb], [psb[b0 + n]])

            def load(j):
                self.fw.dma(self.sp, self.xsrc[j % 2], xt[j % 2][0][:], xin[j * 128:(j + 1) * 128, :], writes=[xt[j % 2][1]])
            load(0)
            for j in range(ntiles):
                xtile, xb_ = xt[j % 2]
                if j + 1 < ntiles:
                    load(j + 1)
                self.norm_T(xtile[:], xb_, gcol[:], gcolb, hT[:], hTb, ntmp)
                proj(1024, 0)
                proj(2048, 2)
                self.A(lambda e: e.activation(out=fS[:], in_=p01, func=AF.Sigmoid), [psb[0], psb[1]], [fSb])
                self.A(lambda e: e.copy(out=v[:], in_=p23), [psb[2], psb[3]], [vb])
                self.V(lambda e: e.tensor_tensor(out=fS[:], in0=fS[:], in1=OML[:], op=ALU.mult), [fSb, OMLb], [fSb])
                self.V(lambda e: e.tensor_tensor(out=fS[:], in0=fS[:], in1=LB[:], op=ALU.add), [fSb, LBb], [fSb])
                self.A(lambda e: e.activation(out=lg[:], in_=fS[:], func=AF.Ln), [fSb], [lgb])
                self.V(lambda e: e.tensor_scalar(out=kf[:], in0=fS[:], scalar1=-1.0, scalar2=1.0, op0=ALU.mult, op1=ALU.add), [fSb], [kfb])
                for n in range(2):
                    self.M(lambda e: e.matmul(self.ps(2 + n), lhsT=triu[:], rhs=lg[:, n * 512:(n + 1) * 512], start=True, stop=True), [triub, lgb], [psb[2 + n]])
                if not state_only:
                    for n in range(2):
                        self.M(lambda e: e.matmul(self.ps(n), lhsT=tri[:], rhs=lg[:, n * 512:(n + 1) * 512], start=True, stop=True), [trib, lgb], [psb[n]])
                for h in range(8):
                    self.M(lambda e: e.matmul(self.ps(4)[:, h * 64:(h + 1) * 64], lhsT=lg[:, h * 128:(h + 1) * 128], rhs=cind[:], start=True, stop=True), [lgb, cindb], [psb[4]])
                self.A(lambda e: e.activation(out=e3[:], in_=p23, func=AF.Exp), [psb[2], psb[3]], [e3b])
                self.A(lambda e: e.activation(out=dec[:].rearrange("p h c -> p (h c)"), in_=self.ps(4), func=AF.Exp), [psb[4]], [decb])
                self.V(lambda e: e.tensor_tensor(out=khat[:], in0=kf[:], in1=e3[:], op=ALU.mult), [kfb, e3b], [khatb])
                if not state_only:
                    self.A(lambda e: e.activation(out=e1[:], in_=p01, func=AF.Exp), [psb[0], psb[1]], [e1b])
                    self.A(lambda e: e.activation(out=e2[:], in_=p01, func=AF.Exp, scale=-1.0), [psb[0], psb[1]], [e2b])
                    proj(0, 5)
                    self.V(lambda e: e.scalar_tensor_tensor(out=qt[:], in0=p56, scalar=float(128 ** -0.5), in1=e1[:], op0=ALU.mult, op1=ALU.mult), [psb[5], psb[6], e1b], [qtb])
                    self.V(lambda e: e.tensor_tensor(out=ktl[:], in0=kf[:], in1=e2[:], op=ALU.mult), [kfb, e2b], [ktlb])
                    pb7 = self.psbf(7)
                    for (src_, srcb_, dst_, dstb_) in ((qt, qtb, qT, qTb), (ktl, ktlb, kT, kTb)):
                        for h in range(8):
                            self.M(lambda e: e.transpose(out=pb7[:, h * 128:(h + 1) * 128], in_=src_[:, h * 128:(h + 1) * 128], identity=ident[:]), [srcb_, identb], [psb[7]])
                        self.A(lambda e: e.copy(out=dst_[:].rearrange("p h t -> p (h t)"), in_=pb7), [psb[7]], [dstb_])
                    proj(3072, 5)
                    self.A(lambda e: e.activation(out=gs[:], in_=p56, func=AF.Silu), [psb[5], psb[6]], [gsb])
                    self.G(lambda e: e.tensor_tensor(out=gs[:].rearrange("p (h d) -> p h d", h=8), in0=gs[:].rearrange("p (h d) -> p h d", h=8), in1=NG[:].unsqueeze(1).to_broadcast([128, 8, 128]), op=ALU.mult), [gsb, NGb], [gsb])
                for h in range(8):
                    hc = slice(h * 128, (h + 1) * 128)
                    if not state_only:
                        amt, amb = Am[h % 2]
                        ob = 5 + h // 4
                        po = self.ps(ob)[:, (h % 4) * 128:(h % 4 + 1) * 128]
                        self.M(lambda e: e.matmul(self.ps(0)[:, 0:128], lhsT=kT[:, h, :], rhs=qT[:, h, :], start=True, stop=True), [kTb, qTb], [psb[0]])
                        self.V(lambda e: e.tensor_tensor(out=amt[:], in0=self.ps(0)[:, 0:128], in1=tri[:], op=ALU.mult), [psb[0], trib], [amb])
                    for c in range(2):
                        rows = slice(c * 64, (c + 1) * 64)
                        pS = self.ps(1 + c)[:, 0:128]
                        self.M(lambda e: e.matmul(pS, lhsT=khat[rows, hc], rhs=v[rows, hc], start=True, stop=True), [khatb, vb], [psb[1 + c]])
                        self.V(lambda e: e.scalar_tensor_tensor(out=S[:, h, :], in0=S[:, h, :], scalar=dec[:, h, c:c + 1], in1=pS, op0=ALU.mult, op1=ALU.add), [Sb_, decb, psb[1 + c]], [Sb_])
                        if c == 0:
                            self.A(lambda e: e.copy(out=Shm[:, h, :], in_=S[:, h, :]), [Sb_], [Shmb])
                            if not state_only:
                                self.M(lambda e: e.matmul(po, lhsT=amt[:], rhs=v[:, hc], start=True, stop=False), [amb, vb], [psb[ob]])
                                self.M(lambda e: e.matmul(po[0:64, :], lhsT=qT[:, h, 0:64], rhs=Sh[:, h, :], start=False, stop=True), [qTb, Shb], [psb[ob]])
                                self.M(lambda e: e.matmul(po[64:128, :], lhsT=qT[:, h, 64:128], rhs=Shm[:, h, :], start=False, stop=True), [qTb, Shmb], [psb[ob]])
                        else:
                            self.A(lambda e: e.copy(out=Sh[:, h, :], in_=S[:, h, :]), [Sb_], [Shb])
                if state_only:
                    continue
                self.A(lambda e: e.activation(out=e1[:], in_=p56, func=AF.Square), [psb[5], psb[6]], [e1b])
                self.V(lambda e: e.tensor_reduce(out=st[:, 8:16], in_=e1[:].rearrange("p (h d) -> p h d", h=8), axis=AX.X, op=ALU.add), [e1b], [stb])
                self.G(lambda e: e.tensor_scalar(out=st[:, 16:24], in0=st[:, 8:16], scalar1=1.0 / 128.0, scalar2=EPS, op0=ALU.mult, op1=ALU.add), [stb], [stb])
                self.G(lambda e: e.tensor_tensor(out=st[:, 24:32], in0=st[:, 16:24], in1=cst[:, 8:16], op=ALU.pow), [stb, cstb], [stb])
                self.V(lambda e: e.tensor_tensor(out=e2[:].rearrange("p (h d) -> p h d", h=8), in0=p56.rearrange("p (h d) -> p h d", h=8), in1=st[:, 24:32].unsqueeze(2).to_broadcast([128, 8, 128]), op=ALU.mult), [psb[5], psb[6], stb], [e2b])
                self.V(lambda e: e.tensor_tensor(out=ocb[:], in0=e2[:], in1=gs[:], op=ALU.mult), [e2b, gsb], [ocbb])
                pb7 = self.psbf(7)
                for h in range(8):
                    self.M(lambda e: e.transpose(out=pb7[:, h * 128:(h + 1) * 128], in_=ocb[:, h * 128:(h + 1) * 128], identity=ident[:]), [ocbb, identb], [psb[7]])
                self.A(lambda e: e.copy(out=ocT[:].rearrange("p h t -> p (h t)"), in_=pb7), [psb[7]], [ocTb])
                for n in range(2):
                    for k in range(8):
                        self.M(lambda e: e.matmul(self.ps(2 + n), lhsT=ocT[:, k, :], rhs=woc[:, k, n * 512:(n + 1) * 512], start=(k == 0), stop=(k == 7)), [ocTb, wocb], [psb[2 + n]])
                self.V(lambda e: e.tensor_tensor(out=xtile[:], in0=xtile[:], in1=p23, op=ALU.add), [xb_, psb[2], psb[3]], [xb_])
                self.fw.dma(self.sp, self.ssrc[j % 2], xout[j * 128:(j + 1) * 128, :], xtile[:], reads=[xb_])
            if send is not None:
                self.fw.dma(self.sp, self.ssrc[0], send, S[:], reads=[Sb_])
            self.fw.barrier()
        self.es = None

def host_consts(half):
    c = {}
    c["ident"] = np.eye(128, dtype=np.float32)
    t = np.arange(128)
    c["cmask"] = np.where(t[None, :] <= t[:, None], 0.0, NEG).astype(np.float32)
    bands = np.zeros((16, 128, 128), np.float32)
    for g, w in enumerate((2, 4, 8, 16)):
        s_ = t[:, None]
        t_ = t[None, :]
        cur = ((s_ <= t_) & (s_ > t_ - w)).astype(np.float32)
        prev = ((s_ - 128) > (t_ - w)).astype(np.float32)
        bands[g] = cur / w - np.eye(128, dtype=np.float32)
        bands[4 + g] = prev / w
        if half == 0:
            cnt = np.minimum(t_ + 1, w).astype(np.float32)
            bands[8 + g] = cur / cnt - np.eye(128, dtype=np.float32)
            bands[12 + g] = 0.0
        else:
            bands[8 + g] = bands[g]
            bands[12 + g] = bands[4 + g]
    c["bands"] = np.ascontiguousarray(bands.transpose(1, 0, 2))
    inv = (np.float32(500000.0) ** (-(np.arange(0, 16, 2, dtype=np.float32)) / np.float32(16))).astype(np.float32)
    c["ropeinv"] = np.ascontiguousarray(np.broadcast_to(inv[None, :], (128, 8))).astype(np.float32)
    c["pow2"] = np.ascontiguousarray(np.broadcast_to((2.0 ** -np.arange(NIT + 1))[None, :], (128, NIT + 1))).astype(np.float32)
    c["prevbias"] = np.full((128, 1), NEG if half == 0 else 0.0, np.float32)
    return c


def col_layout(v, p=128):
    v = np.asarray(v)
    return np.ascontiguousarray(v.reshape(-1, p).T)


def build_a(ntiles=NT, nprev=NT, stop=99):
    nc = bass.Bass("TRN2", target_bir_lowering=False)
    kb = KB(nc)
    x_own = kb.din("x_own", [L_OWN, D])
    x_prev = kb.din("x_prev", [L_OWN, D])
    for name, shape, dt in (("ident", [128, 128], F32), ("cmask", [128, 128], F32), ("bands", [128, 16, 128], F32),
                            ("ropeinv", [128, 8], F32), ("pow2", [128, NIT + 1], F32), ("prevbias", [128, 1], F32),
                            ("pos", [128, NKT], I32), ("g_mix0", [128, 8], F32), ("pscale", [128, 4], F32),
                            ("idx_k_norm", [64], F32), ("w_in_ab", [D, AB_IN], F32), ("w_out_ab", [D, D], F32),
                            ("w_pool", [4, 128, 128], F32)):
        kb.din(name, shape, dt)
    xmid = kb.dout("xmid", [L_OWN, D])
    kb.phase_a(x_own, x_prev, xmid, ntiles=ntiles, nprev=nprev, stop=stop)
    return nc, kb


def inputs_a(inp, core, nprev=NT):
    b, half = core // 2, core % 2
    c = host_consts(half)
    x = inp["x"]
    m = {}
    m["x_own"] = np.ascontiguousarray(x[b, half * L_OWN:(half + 1) * L_OWN])
    m["x_prev"] = np.ascontiguousarray(x[b, 0:L_OWN])
    pos = inp["positions"][b]
    posw = np.concatenate([pos[0:nprev * 128], pos[half * L_OWN:(half + 1) * L_OWN], pos[0:(NT - nprev) * 128]]).astype(np.int32)
    m["pos"] = np.ascontiguousarray(posw.reshape(NKT, 128).T)
    for k in ("ident", "cmask", "bands", "ropeinv", "pow2", "prevbias"):
        m[k] = c[k]
    m["g_mix0"] = col_layout(inp["ln_mix"][0])
    m["pscale"] = col_layout(inp["pool_scale"][0])
    m["idx_k_norm"] = np.ascontiguousarray(inp["idx_k_norm"][0])
    m["w_in_ab"] = np.ascontiguousarray(inp["w_in_ab"][0])
    m["w_out_ab"] = np.ascontiguousarray(inp["w_out_ab"][0])
    m["w_pool"] = np.ascontiguousarray(inp["w_pool"][0])
    return m


def build_ffn(sfx, final, ntiles=NT):
    nc = bass.Bass("TRN2", target_bir_lowering=False)
    kb = KB(nc)
    xin = kb.din("xin", [L_OWN, D])
    kb.din("ident", [128, 128])
    kb.din("g_ffn" + sfx, [128, 8])
    kb.din("w_gate_up" + sfx, [D, 2 * DFF])
    kb.din("w_down" + sfx, [DFF, D])
    if final:
        kb.din("ln_final", [D])
    xout = kb.dout("xout", [L_OWN, D])
    kb.phase_ffn(xin, xout, sfx, final=final, ntiles=ntiles)
    return nc, kb


def inputs_ffn(inp, layer, xin, final):
    sfx = str(layer)
    m = {"xin": np.ascontiguousarray(xin), "ident": np.eye(128, dtype=np.float32),
         "g_ffn" + sfx: col_layout(inp["ln_ffn"][layer]),
         "w_gate_up" + sfx: np.ascontiguousarray(inp["w_gate_up"][layer]),
         "w_down" + sfx: np.ascontiguousarray(inp["w_down"][layer])}
    if final:
        m["ln_final"] = np.ascontiguousarray(inp["ln_final"])
    return m


def hgrn_consts():
    t = np.arange(128)
    same = (t[:, None] // 64) == (t[None, :] // 64)
    c = {}
    c["tribd"] = (same & (t[:, None] <= t[None, :])).astype(np.float32)
    c["triu"] = (same & (t[:, None] > t[None, :])).astype(np.float32)
    ci = np.zeros((128, 64), np.float32)
    ci[:64, 0] = 1.0
    ci[64:, 1] = 1.0
    c["cind"] = ci
    return c


def build_hgrn(state_only, ntiles=NT):
    nc = bass.Bass("TRN2", target_bir_lowering=False)
    kb = KB(nc)
    xin = kb.din("xin", [L_OWN, D])
    sinit = kb.din("sinit", [128, 8, 128])
    for name, shape in (("ident", [128, 128]), ("g_mix1", [128, 8]), ("tribd", [128, 128]), ("triu", [128, 128]),
                        ("cind", [128, 64]), ("hgrn_lb", [2, 1024]), ("w_in_c", [D, 4096])):
        kb.din(name, shape)
    if not state_only:
        kb.din("w_out_c", [D, D])
        kb.din("hgrn_out_norm", [128])
        xout = kb.dout("xout", [L_OWN, D])
        send = None
    else:
        xout = None
        send = kb.dout("send", [128, 8, 128])
    kb.phase_hgrn(xin, xout, sinit, send, state_only, ntiles=ntiles)
    return nc, kb


def inputs_hgrn(inp, xin, sinit, state_only):
    m = {"xin": np.ascontiguousarray(xin), "sinit": np.ascontiguousarray(sinit), "ident": np.eye(128, dtype=np.float32),
         "g_mix1": col_layout(inp["ln_mix"][1]), "hgrn_lb": np.ascontiguousarray(inp["hgrn_lb"]),
         "w_in_c": np.ascontiguousarray(inp["w_in_c"][0])}
    m.update(hgrn_consts())
    if not state_only:
        m["w_out_c"] = np.ascontiguousarray(inp["w_out_c"][0])
        m["hgrn_out_norm"] = np.ascontiguousarray(inp["hgrn_out_norm"][0])
    return m


def _run(nc, maps):
    res = run_bass_kernel_spmd(nc, maps, core_ids=list(range(NCORES)))
    return res.results


def kernel(**inputs):
    inp = {k: np.asarray(v) for k, v in inputs.items()}
    cores = list(range(NCORES))
    nc, _ = build_a()
    r = _run(nc, [inputs_a(inp, c) for c in cores])
    xmid0 = [r[c]["xmid"] for c in cores]
    nc, _ = build_ffn("0", False)
    r = _run(nc, [inputs_ffn(inp, 0, xmid0[c], False) for c in cores])
    x1 = [r[c]["xout"] for c in cores]
    del xmid0
    zero_s = np.zeros((128, 8, 128), np.float32)
    nc, _ = build_hgrn(True)
    r = _run(nc, [inputs_hgrn(inp, x1[c], zero_s, True) for c in cores])
    send = [r[c]["send"] for c in cores]
    nc, _ = build_hgrn(False)
    r = _run(nc, [inputs_hgrn(inp, x1[c], zero_s if c % 2 == 0 else send[c - 1], False) for c in cores])
    xmid1 = [r[c]["xout"] for c in cores]
    del x1
    nc, _ = build_ffn("1", True)
    r = _run(nc, [inputs_ffn(inp, 1, xmid1[c], True) for c in cores])
    out = np.empty((4, 2 * L_OWN, D), np.float32)
    for c in cores:
        out[c // 2, (c % 2) * L_OWN:(c % 2 + 1) * L_OWN] = r[c]["xout"]
    return out
```
